# Optimizing a Trainium2 kernel written in Bass

```python
import jax, jax.numpy as jnp
from jax import lax
import numpy as np

D_MODEL = 1024
BATCH = 2
SEQ = 8192
DEPTH = 1

SB_HEADS = 8
SB_HEAD_DIM = 64
SB_BLOCK = 128
SB_W = SB_HEADS * SB_HEAD_DIM
GDN_HEADS = 8
GDN_DK = 64
GDN_DV = 64
GDN_CONV = 4
GDN_CHUNK = 64
GDN_QK_W = GDN_HEADS * GDN_DK
GDN_V_W = GDN_HEADS * GDN_DV
GDN_CONV_W = 2 * GDN_QK_W + GDN_V_W
IN_WIDTHS = (SB_W, SB_W, SB_W, GDN_CONV_W, GDN_HEADS, GDN_HEADS, GDN_V_W, 2 * D_MODEL)
IN_W = sum(IN_WIDTHS)
PEER_HEADS = 8
PEER_NKEYS = 128
PEER_N_EXPERTS = PEER_NKEYS * PEER_NKEYS
PEER_QDIM = 256
PEER_HALF = PEER_QDIM // 2
PEER_TOPK = 16
PEER_BLOCK = 128
EPS = 1e-6

kernel_name = "hybrid_sb_gdn_peer_block"


def rmsnorm(x, w):
    x32 = x.astype(jnp.float32)
    y = x32 * lax.rsqrt(jnp.mean(x32 * x32, axis=-1, keepdims=True) + EPS) * w.astype(jnp.float32)
    return y.astype(x.dtype)


def l2norm(x):
    return x * lax.rsqrt(jnp.sum(x * x, axis=-1, keepdims=True) + EPS)


def modulate(h, shift, scale):
    return h * (1.0 + scale[:, None, :]) + shift[:, None, :]


def stick_breaking_attention(q, k, v):
    B, S, H, d = q.shape
    nb = S // SB_BLOCK
    qh = q.astype(jnp.float32).transpose(0, 2, 1, 3) * (d ** -0.5)
    kh = k.astype(jnp.float32).transpose(0, 2, 1, 3)
    vh = v.astype(jnp.float32).transpose(0, 2, 1, 3)
    qb = qh.reshape(B, H, nb, SB_BLOCK, d).transpose(2, 0, 1, 3, 4)
    key_pos = jnp.arange(S)

    def block(args):
        q_blk, start = args
        z = jnp.einsum('bhqd,bhkd->bhqk', q_blk, kh)
        qpos = start + jnp.arange(SB_BLOCK)
        causal = key_pos[None, :] < qpos[:, None]
        log_keep = jnp.where(causal, jax.nn.log_sigmoid(-z), 0.0)
        log_stick = lax.cumsum(log_keep, axis=3, reverse=True) - log_keep
        attn = jnp.where(causal, jnp.exp(jax.nn.log_sigmoid(z) + log_stick), 0.0)
        return jnp.einsum('bhqk,bhkd->bhqd', attn, vh)

    starts = jnp.arange(nb) * SB_BLOCK
    out = lax.map(block, (qb, starts))
    out = out.transpose(1, 0, 3, 2, 4).reshape(B, S, H * d)
    return out.astype(q.dtype)


def chunk_gated_delta_rule(q, k, v, g, beta):
    B, H, L, dk = q.shape
    dv = v.shape[-1]
    C = GDN_CHUNK
    N = L // C
    q = q.reshape(B, H, N, C, dk)
    k = k.reshape(B, H, N, C, dk)
    v = v.reshape(B, H, N, C, dv)
    g = g.reshape(B, H, N, C)
    beta = beta.reshape(B, H, N, C)
    G = jnp.cumsum(g, axis=-1)
    idx = jnp.arange(C)
    incl = idx[:, None] >= idx[None, :]
    strict = idx[:, None] > idx[None, :]
    decay = jnp.exp(jnp.where(incl, G[..., :, None] - G[..., None, :], -jnp.inf))
    kk = jnp.einsum('bhncd,bhnsd->bhncs', k, k)
    A = jnp.where(strict, beta[..., :, None] * kk * decay, 0.0)
    M = jnp.eye(C, dtype=jnp.float32) + A
    rhs = jnp.concatenate([v * beta[..., None], k * (beta * jnp.exp(G))[..., None]], axis=-1)
    sol = lax.linalg.triangular_solve(M, rhs, left_side=True, lower=True, unit_diagonal=True)
    u, w = sol[..., :dv], sol[..., dv:]
    qk = jnp.einsum('bhncd,bhnsd->bhncs', q, k) * decay
    q_dec = q * jnp.exp(G)[..., None]
    k_dec = k * jnp.exp(G[..., -1:] - G)[..., None]
    chunk_decay = jnp.exp(G[..., -1])

    def step(S, inp):
        u_c, w_c, qk_c, qd_c, kd_c, cd_c = inp
        v_new = u_c - jnp.einsum('bhcd,bhde->bhce', w_c, S)
        o = jnp.einsum('bhcd,bhde->bhce', qd_c, S) + jnp.einsum('bhcs,bhse->bhce', qk_c, v_new)
        S = S * cd_c[..., None, None] + jnp.einsum('bhcd,bhce->bhde', kd_c, v_new)
        return S, o

    xs = tuple(jnp.moveaxis(t, 2, 0) for t in (u, w, qk, q_dec, k_dec, chunk_decay))
    S0 = jnp.zeros((B, H, dk, dv), jnp.float32)
    _, o = lax.scan(step, S0, xs)
    return jnp.moveaxis(o, 0, 2).reshape(B, H, L, dv)


def gated_deltanet(qkv, a, b, z, conv_w, A_log, dt_bias, o_norm_w):
    B, S, _ = qkv.shape
    dtype = qkv.dtype
    conv = lax.conv_general_dilated(
        qkv.astype(jnp.float32), conv_w.astype(jnp.float32)[:, None, :],
        window_strides=(1,), padding=[(GDN_CONV - 1, 0)],
        dimension_numbers=('NWC', 'WIO', 'NWC'), feature_group_count=GDN_CONV_W)
    conv = jax.nn.silu(conv)
    q = conv[..., :GDN_QK_W].reshape(B, S, GDN_HEADS, GDN_DK)
    k = conv[..., GDN_QK_W:2 * GDN_QK_W].reshape(B, S, GDN_HEADS, GDN_DK)
    v = conv[..., 2 * GDN_QK_W:].reshape(B, S, GDN_HEADS, GDN_DV)
    q = l2norm(q) * (GDN_DK ** -0.5)
    k = l2norm(k)
    beta = jax.nn.sigmoid(b.astype(jnp.float32))
    g = -jnp.exp(A_log.astype(jnp.float32)) * jax.nn.softplus(a.astype(jnp.float32) + dt_bias.astype(jnp.float32))
    o = chunk_gated_delta_rule(q.transpose(0, 2, 1, 3), k.transpose(0, 2, 1, 3), v.transpose(0, 2, 1, 3),
                               g.transpose(0, 2, 1), beta.transpose(0, 2, 1))
    o = o.transpose(0, 2, 1, 3)
    o = rmsnorm(o, o_norm_w) * jax.nn.silu(z.astype(jnp.float32).reshape(B, S, GDN_HEADS, GDN_DV))
    return o.reshape(B, S, GDN_V_W).astype(dtype)


def peer_ffn(h, w_q, sub_keys, u_tab, v_tab):
    B, S, D = h.shape
    T = B * S
    hf = h.reshape(T, D)
    q = (hf @ w_q).astype(jnp.float32).reshape(T, PEER_HEADS, 2, PEER_HALF)
    s = jnp.einsum('thpd,hpkd->thpk', q, sub_keys.astype(jnp.float32))
    s1, i1 = lax.top_k(s[:, :, 0], PEER_TOPK)
    s2, i2 = lax.top_k(s[:, :, 1], PEER_TOPK)
    cand = (s1[..., :, None] + s2[..., None, :]).reshape(T, PEER_HEADS, PEER_TOPK * PEER_TOPK)
    cidx = (i1[..., :, None] * PEER_NKEYS + i2[..., None, :]).reshape(T, PEER_HEADS, PEER_TOPK * PEER_TOPK)
    top, pos = lax.top_k(cand, PEER_TOPK)
    eidx = jnp.take_along_axis(cidx, pos, axis=-1).reshape(T, PEER_HEADS * PEER_TOPK)
    gate = jax.nn.softmax(top, axis=-1).reshape(T, PEER_HEADS * PEER_TOPK)
    nb = T // PEER_BLOCK

    def block(args):
        h_b, idx_b, g_b = args
        u_sel = u_tab[idx_b]
        act = jax.nn.gelu(jnp.einsum('tkd,td->tk', u_sel, h_b).astype(jnp.float32), approximate=False)
        coef = (g_b * act).astype(h_b.dtype)
        return jnp.einsum('tk,tkd->td', coef, v_tab[idx_b])

    out = lax.map(block, (hf.reshape(nb, PEER_BLOCK, D),
                          eidx.reshape(nb, PEER_BLOCK, PEER_HEADS * PEER_TOPK),
                          gate.reshape(nb, PEER_BLOCK, PEER_HEADS * PEER_TOPK)))
    return out.reshape(B, S, D)


def setup_inputs(seed: int = 0) -> dict:
    key = jax.random.key(seed)
    ks = jax.random.split(key, 24)
    f32 = jnp.float32
    D = D_MODEL
    nrm = lambda k, shape, s: jax.random.normal(k, shape, f32) * s
    dt = jnp.exp(jax.random.uniform(ks[10], (DEPTH, GDN_HEADS), f32, np.log(1e-3), np.log(1e-1)))
    return {
        "x": nrm(ks[0], (BATCH, SEQ, D), 1.0),
        "c": nrm(ks[1], (BATCH, D), 1.0),
        "w_ada": nrm(ks[2], (DEPTH, D, 6 * D), 0.5 * D ** -0.5),
        "b_ada": nrm(ks[3], (DEPTH, 6 * D), 0.02),
        "norm1_w": 1.0 + nrm(ks[4], (DEPTH, D), 0.02),
        "w_in": nrm(ks[5], (DEPTH, D, IN_W), D ** -0.5),
        "sb_q_norm_w": 1.0 + nrm(ks[6], (DEPTH, SB_HEAD_DIM), 0.02),
        "sb_k_norm_w": 1.0 + nrm(ks[7], (DEPTH, SB_HEAD_DIM), 0.02),
        "gdn_conv_w": nrm(ks[8], (DEPTH, GDN_CONV, GDN_CONV_W), 0.5),
        "gdn_A_log": jnp.log(jax.random.uniform(ks[9], (DEPTH, GDN_HEADS), f32, 1.0, 16.0)),
        "gdn_dt_bias": dt + jnp.log(-jnp.expm1(-dt)),
        "gdn_o_norm_w": 1.0 + nrm(ks[11], (DEPTH, GDN_DV), 0.02),
        "w_proj_sb": nrm(ks[12], (DEPTH, SB_W, D), SB_W ** -0.5),
        "w_proj_gdn": nrm(ks[13], (DEPTH, GDN_V_W, D), GDN_V_W ** -0.5),
        "w_o": nrm(ks[14], (DEPTH, D, D), D ** -0.5),
        "norm2_w": 1.0 + nrm(ks[15], (DEPTH, D), 0.02),
        "peer_w_q": nrm(ks[16], (DEPTH, D, PEER_HEADS * PEER_QDIM), D ** -0.5),
        "peer_sub_keys": nrm(ks[17], (DEPTH, PEER_HEADS, 2, PEER_NKEYS, PEER_HALF), PEER_HALF ** -0.5),
        "peer_u": nrm(ks[18], (DEPTH, PEER_N_EXPERTS, D), D ** -0.5),
        "peer_v": nrm(ks[19], (DEPTH, PEER_N_EXPERTS, D), PEER_HEADS ** -0.5),
    }


def reference(x, c, w_ada, b_ada, norm1_w, w_in, sb_q_norm_w, sb_k_norm_w, gdn_conv_w, gdn_A_log,
              gdn_dt_bias, gdn_o_norm_w, w_proj_sb, w_proj_gdn, w_o, norm2_w, peer_w_q, peer_sub_keys,
              peer_u, peer_v):
    B, S, D = x.shape
    split_at = np.cumsum(IN_WIDTHS)[:-1].tolist()
    for l in range(DEPTH):
        mod = (jax.nn.silu(c) @ w_ada[l] + b_ada[l]).reshape(B, 6, D)
        shift1, scale1, gate1, shift2, scale2, gate2 = [mod[:, i] for i in range(6)]

        h = modulate(rmsnorm(x, norm1_w[l]), shift1, scale1)
        proj = h @ w_in[l]
        sb_q, sb_k, sb_v, gdn_qkv, gdn_a, gdn_b, gdn_z, gate_logits = jnp.split(proj, split_at, axis=-1)
        sb_q = rmsnorm(sb_q.reshape(B, S, SB_HEADS, SB_HEAD_DIM), sb_q_norm_w[l])
        sb_k = rmsnorm(sb_k.reshape(B, S, SB_HEADS, SB_HEAD_DIM), sb_k_norm_w[l])
        sb_v = sb_v.reshape(B, S, SB_HEADS, SB_HEAD_DIM)
        y_sb = stick_breaking_attention(sb_q, sb_k, sb_v)
        y_gdn = gated_deltanet(gdn_qkv, gdn_a, gdn_b, gdn_z, gdn_conv_w[l], gdn_A_log[l],
                               gdn_dt_bias[l], gdn_o_norm_w[l])
        gates = jax.nn.sigmoid(gate_logits.astype(jnp.float32)).astype(x.dtype).reshape(B, S, 2, D)
        merged = gates[:, :, 0] * (y_sb @ w_proj_sb[l]) + gates[:, :, 1] * (y_gdn @ w_proj_gdn[l])
        x = x + gate1[:, None, :] * (merged @ w_o[l])

        h2 = modulate(rmsnorm(x, norm2_w[l]), shift2, scale2)
        x = x + gate2[:, None, :] * peer_ffn(h2, peer_w_q[l], peer_sub_keys[l], peer_u[l], peer_v[l])
    return x
```

```python
import numpy as np
import contextlib
import concourse.bass as bass
import concourse.mybir as mybir
from concourse.bass_utils import run_bass_kernel_spmd

F32 = mybir.dt.float32
BF16 = mybir.dt.bfloat16
U32 = mybir.dt.uint32
I32 = mybir.dt.int32
ALU = mybir.AluOpType
AF = mybir.ActivationFunctionType
AX = mybir.AxisListType


class Buf:
    __slots__ = ("name", "w", "r", "excl")

    def __init__(self, name=""):
        self.name = name
        self.w = None
        self.r = {}
        self.excl = False


class _Eng:
    def __init__(self, name, eng, sem):
        self.name = name
        self.eng = eng
        self.sem = sem
        self.count = 0
        self.waited = {}
        self.ops = []


class Sched:
    def __init__(self, nc, stack, n_dma_sems=24):
        self.nc = nc
        self.E = {}
        for name, eng in (("pe", nc.tensor), ("act", nc.scalar), ("dve", nc.vector),
                          ("pool", nc.gpsimd), ("sp", nc.sync)):
            sem = stack.enter_context(nc.semaphore("sem_" + name))
            self.E[name] = _Eng(name, eng, sem)
        self.dsems = [[stack.enter_context(nc.semaphore(f"dsem{i}")), 0] for i in range(n_dma_sems)]
        self.dnext = 0
        self.dsems_sw = [[stack.enter_context(nc.semaphore(f"swsem{i}")), 0] for i in range(12)]
        self.dnext_sw = 0
        self.nops = 0

    def _deps(self, E, reads, writes, skip_own):
        deps = {}

        def add(d):
            if d is None:
                return
            k = id(d[0])
            if k not in deps or deps[k][1] < d[1]:
                deps[k] = d

        for b in reads:
            add(b.w)
            if b.excl:
                for k_, d in b.r.items():
                    if k_ != id(E.sem):
                        add(d)
        for b in writes:
            add(b.w)
            for d in b.r.values():
                add(d)
        waits = []
        for k, (sem, v) in deps.items():
            if skip_own and sem is E.sem:
                continue
            if E.waited.get(k, 0) < v:
                E.waited[k] = v
                waits.append((sem, v))
        return waits

    def _mark(self, me, reads, writes):
        k = id(me[0])
        for b in reads:
            old = b.r.get(k)
            if old is None or old[1] < me[1]:
                b.r[k] = me
        for b in writes:
            b.w = me
            b.r = {}

    def op(self, en, fn, reads=(), writes=()):
        E = self.E[en]
        waits = self._deps(E, reads, writes, skip_own=(en == "pe"))
        E.count += 1
        me = (E.sem, E.count)
        E.ops.append((waits, fn, E.sem, 1))
        self._mark(me, reads, writes)
        self.nops += 1

    def dma(self, en, fn, reads=(), writes=()):
        E = self.E[en]
        waits = self._deps(E, reads, writes, skip_own=False)
        if en == "pool":
            slot = self.dsems_sw[self.dnext_sw]
            self.dnext_sw = (self.dnext_sw + 1) % len(self.dsems_sw)
        else:
            slot = self.dsems[self.dnext]
            self.dnext = (self.dnext + 1) % len(self.dsems)
        sem, cnt = slot
        if cnt > 0 and E.waited.get(id(sem), 0) < cnt:
            E.waited[id(sem)] = cnt
            waits.append((sem, cnt))
        slot[1] = cnt + 16
        me = (sem, cnt + 16)
        E.ops.append((waits, fn, sem, 16))
        self._mark(me, reads, writes)
        self.nops += 1

    def full_barrier(self):
        allsem = [(E.sem, E.count) for E in self.E.values() if E.count > 0] + [(s_[0], s_[1]) for s_ in (self.dsems + self.dsems_sw) if s_[1] > 0]
        for E in self.E.values():
            waits = []
            for sem, v in allsem:
                if E.waited.get(id(sem), 0) < v:
                    E.waited[id(sem)] = v
                    waits.append((sem, v))
            E.ops.append((waits, None, None, 0))

    def barrier(self, buf):
        for en in self.E:
            self.wait_all(en, [buf])

    def wait_all(self, en, bufs):
        E = self.E[en]
        waits = self._deps(E, bufs, (), skip_own=False)
        E.ops.append((waits, None, None, 0))

    def emit(self):
        nc = self.nc
        with nc.Block() as block:
            def mk(E):
                def body(e):
                    if E.name == "pool":
                        E.core = e.partition_id()
                    for waits, fn, sem, inc in E.ops:
                        for s, v in waits:
                            e.wait_ge(s, v)
                        if fn is not None:
                            ins = fn(e)
                            ins.then_inc(sem, inc)
                return body
            block.tensor(mk(self.E["pe"]))
            block.scalar(mk(self.E["act"]))
            block.vector(mk(self.E["dve"]))
            block.gpsimd(mk(self.E["pool"]))
            block.sync(mk(self.E["sp"]))


import os
import numpy as np
import contextlib
import ml_dtypes

D = 1024
EPS = 1e-6
NQ = 512


def host_consts():
    c = {}
    p = np.arange(128)[:, None]
    f = np.arange(128)[None, :]
    c["ident"] = (p == f).astype(np.float32)
    c["ones"] = np.ones((128, 128), np.float32)
    c["negones"] = -np.ones((128, 128), np.float32)
    c["negtri"] = -(p >= f).astype(np.float32)
    bo = np.zeros((128, 128), np.float32)
    bo[:64, :64] = 1
    bo[64:, 64:] = 1
    c["blockones"] = bo
    p64 = (np.arange(128) % 64)[:, None]
    f64 = np.arange(64)[None, :]
    c["triu"] = (p64 <= f64).astype(np.float32)
    c["ls"] = (p64 > f64).astype(np.float32)
    c["ui"] = (f64 >= p64).astype(np.float32)
    c["id64"] = (p64 == f64).astype(np.float32)
    sh = np.zeros((128, 64), np.float32)
    sh[64 + np.arange(64), np.arange(64)] = 1
    c["shift"] = sh
    t = np.arange(512)[None, :]
    for r in range(4):
        c[f"dmask{r}"] = ((p + 128 * r) < t).astype(np.float32)
    l2s = np.ones((128, 1), np.float32)
    l2s[:64] = 0.125
    c["l2s"] = l2s
    c["eps"] = np.full((128, 1), EPS, np.float32)
    c["one"] = np.ones((128, 1), np.float32)
    c["iota16"] = np.tile(np.arange(16, dtype=np.float32)[None, :], (128, 1))
    off = {}
    cols = 0
    for k, v in c.items():
        off[k] = (cols, v.shape[1])
        cols += v.shape[1]
    arr = np.concatenate(list(c.values()), axis=1)
    return arr, off


CONST_ARR, CONST_OFF = host_consts()


class T:
    def __init__(self, t, name):
        self.t = t
        self.b = Buf(name)

    def __getitem__(self, k):
        return self.t[k]


KEEP = {"cT", "scT", "badaT", "n1w", "modT", "G1"}


def build_A(nc, S, sch, stack, io, outer=None):
    outer = outer or stack
    NB = 2
    TT = NB * S
    NM = TT // NQ
    MPB = S // NQ
    NKB = S // 128

    pre = {}
    for (nm_, shp_) in (("cT", [128, 8, 3]), ("scT", [128, 8, 3]), ("badaT", [128, 48]), ("n1w", [128, 8]),
                        ("modT", [128, 48, 3]), ("G1", [128, 8, 3])):
        pre[nm_] = T(outer.enter_context(nc.sbuf_tensor("s_" + nm_, shp_, F32)), nm_)

    def sb(name, shape, dt):
        if name in pre:
            return pre[name]
        return T(stack.enter_context(nc.sbuf_tensor("s_" + name, shape, dt)), name)

    def ps(name, shape=(128, 512), dt=F32):
        t_ = T(outer.enter_context(nc.psum_tensor("p_" + name, list(shape), dt)), name)
        t_.b.excl = True
        return t_

    op = sch.op
    dma = sch.dma

    NCC = CONST_ARR.shape[1]
    cf = sb("cf", [128, NCC], F32)
    cb = sb("cb", [128, NCC], BF16)
    dma("sp", lambda e: e.dma_start(out=cf[:, :], in_=io["consts"][:, :]), writes=[cf.b])
    op("dve", lambda e: e.tensor_copy(out=cb[:, :], in_=cf[:, :]), reads=[cf.b], writes=[cb.b])

    def CF(k, rows=slice(0, 128)):
        o, n = CONST_OFF[k]
        return cf[rows, o:o + n]

    def CB(k, rows=slice(0, 128)):
        o, n = CONST_OFF[k]
        return cb[rows, o:o + n]

    banks = [ps(f"bank{i}") for i in range(8)]
    rot = [0]

    def bank():
        b = banks[rot[0] % 6]
        rot[0] += 1
        return b

    cT = sb("cT", [128, 8, 3], F32)
    scT = sb("scT", [128, 8, 3], F32)
    badaT = sb("badaT", [128, 48], F32)
    n1w = sb("n1w", [128, 8], F32)
    modT = sb("modT", [128, 48, 3], F32)
    G1 = sb("G1", [128, 8, 3], F32)
    wst = sb("wst", [128, 8, 1024], F32)
    dma("sp", lambda e: e.dma_start(out=cT[:, :, :], in_=io["cT"][:, :, :]), writes=[cT.b])
    dma("sp", lambda e: e.dma_start(out=badaT[:, :], in_=io["badaT"][:, :]), writes=[badaT.b])
    dma("sp", lambda e: e.dma_start(out=n1w[:, :], in_=io["n1wT"][:, :]), writes=[n1w.b])
    op("act", lambda e: e.activation(out=scT[:, :, :], in_=cT[:, :, :], func=AF.Silu), reads=[cT.b], writes=[scT.b])
    pm = banks[6]
    for g in range(6):
        dma("sp", lambda e, g=g: e.dma_start(
            out=wst[:, :, :], in_=io["w_ada"][:, g * 1024:(g + 1) * 1024].rearrange("(c p) n -> p c n", p=128)),
            writes=[wst.b])
        for j in range(8):
            col = (g * 8 + j) * 3
            for kc in range(8):
                op("pe", lambda e, j=j, kc=kc, col=col: e.matmul(
                    pm[:, col:col + 3], lhsT=wst[:, kc, j * 128:(j + 1) * 128], rhs=scT[:, kc, :],
                    start=(kc == 0), stop=(kc == 7)), reads=[wst.b, scT.b], writes=[pm.b])
    op("dve", lambda e: e.tensor_tensor(
        out=modT[:, :, :], in0=pm[:, 0:144].rearrange("p (c j) -> p c j", j=3),
        in1=badaT[:, :].unsqueeze(2).to_broadcast([128, 48, 3]), op=ALU.add),
        reads=[pm.b, badaT.b], writes=[modT.b])
    op("dve", lambda e: e.scalar_tensor_tensor(
        out=G1[:, :, :], in0=modT[:, 8:16, :], scalar=1.0,
        in1=n1w[:, :].unsqueeze(2).to_broadcast([128, 8, 3]), op0=ALU.add, op1=ALU.mult),
        reads=[modT.b, n1w.b], writes=[G1.b])

    STAGE = float(os.environ.get('STAGE', '99'))
    if STAGE < 1:
        return
    wh = sb("wh", [128, 8, 386], BF16)
    dma("sp", lambda e: e.dma_start(
        out=wst[:, :, 0:386], in_=io["w_head"].rearrange("(c p) n -> p c n", p=128)), writes=[wst.b])
    op("dve", lambda e: e.tensor_copy(out=wh[:, :, :], in_=wst[:, :, 0:386]), reads=[wst.b], writes=[wh.b])
    Wqq = sb("Wqq", [128, 8, 128], BF16)
    Wkk = sb("Wkk", [128, 8, 128], BF16)
    Wv66 = sb("Wv66", [128, 8, 66], BF16)
    for dst, lo in ((Wqq, 0), (Wkk, 64)):
        for h2 in range(2):
            op("dve", lambda e, dst=dst, lo=lo, h2=h2: e.tensor_copy(
                out=dst[:, :, h2 * 64:(h2 + 1) * 64], in_=wh[:, :, lo:lo + 64]), reads=[wh.b], writes=[dst.b])
    op("dve", lambda e: e.tensor_copy(out=Wv66[:, :, 0:64], in_=wh[:, :, 128:192]), reads=[wh.b], writes=[Wv66.b])
    op("dve", lambda e: e.tensor_copy(out=Wv66[:, :, 64:66], in_=wh[:, :, 384:386]), reads=[wh.b], writes=[Wv66.b])
    hv = sb("hv", [128, 8], F32)
    hv2 = sb("hv2", [128, 4], F32)
    dma("sp", lambda e: e.dma_start(out=hv[:, :], in_=io["hvec"][:, :]), writes=[hv.b])
    dma("sp", lambda e: e.dma_start(out=hv2[:, :], in_=io["hvec2"][:, :]), writes=[hv2.b])
    wq8 = sb("wq8", [128, 1], F32)
    negA = sb("negA", [128, 1], F32)
    op("dve", lambda e: e.tensor_scalar(out=wq8[:, :], in0=hv[:, 0:1], scalar1=0.125, scalar2=None, op0=ALU.mult),
       reads=[hv.b], writes=[wq8.b])
    op("act", lambda e: e.activation(out=negA[:, :], in_=hv[:, 2:3], func=AF.Exp), reads=[hv.b], writes=[negA.b])
    op("dve", lambda e: e.tensor_scalar(out=negA[:, :], in0=negA[:, :], scalar1=-1.0, scalar2=None, op0=ALU.mult),
       reads=[negA.b], writes=[negA.b])

    if STAGE < 2:
        return
    KT = sb("KT", [128, S], BF16)
    KTb = [Buf(f"KT{m}") for m in range(NM)]
    Vt = sb("Vt", [128, NB * NKB, 64], BF16)
    Vtb = [Buf(f"Vt{m}") for m in range(NM)]
    qT = sb("qT", [128, NQ], BF16)
    xt = [sb(f"xt{i}", [128, D], F32) for i in range(2)]
    xn = [sb(f"xn{i}", [128, D], BF16) for i in range(2)]
    junk = sb("junk", [128, D], BF16)
    ssq = sb("ssq", [128, 1], F32)
    rstd = sb("rstd", [128, 1], F32)
    hT = sb("hT", [128, 8, NQ], BF16)
    sqb = sb("sqb", [128, NQ], BF16)
    rt = sb("rt", [128, NQ], F32)
    Esb = sb("Esb", [128, NQ], F32)
    Lp = [sb(f"Lp{i}", [128, NQ], BF16) for i in range(2)]
    Lacc = sb("Lacc", [128, NQ], BF16)
    At = [sb(f"At{i}", [128, NQ], BF16) for i in range(2)]
    ysb = sb("ysb", [64, NQ], BF16)
    OT = banks[7]
    cq = sb("cq", [128, NQ + 3], F32)
    cv = sb("cv", [64, NQ + 3], F32)
    acc = sb("acc", [128, NQ], F32)
    accv = sb("accv", [64, NQ], F32)
    sq = sb("sq", [128, NQ], F32)
    sqq = sb("sqq", [128, NQ], F32)
    qkn = sb("qkn", [128, NQ], F32)
    kT0 = sb("kT0", [64, NQ], F32)
    vT0 = sb("vT0", [64, NQ], F32)
    gb = sb("gb", [128, 2, 4], F32)
    tmpab = sb("tmpab", [128, 4], F32)
    gsc = sb("gsc", [64, 2, 8], F32)
    Gcol = sb("Gcol", [64, 8], F32)
    GLs = sb("GLs", [64, 8], F32)
    expG = sb("expG", [64, 8], F32)
    bexpG = sb("bexpG", [64, 8], F32)
    kdecs = sb("kdecs", [64, 8], F32)
    cd = sb("cd", [64, 8], F32)
    diagG = sb("diagG", [64, 8, 64], F32)
    negD = sb("negD", [64, 8, 64], F32)
    e1 = sb("e1", [64, 8, 64], F32)
    e2 = sb("e2", [64, 8, 64], F32)
    eR = sb("eR", [64, NQ], F32)
    Rsb = sb("Rsb", [64, NQ], F32)
    qdT = sb("qdT", [64, NQ], F32)
    Ak = [sb(f"Ak{i}", [64, 8, 64], F32) for i in range(2)]
    Bk = [sb(f"Bk{i}", [64, 8, 64], F32) for i in range(2)]
    Xk = [sb(f"Xk{i}", [64, 8, 64], F32) for i in range(2)]
    qkT = sb("qkT", [64, 8, 64], F32)
    kbm = sb("kbm", [64, 8, 64], F32)
    kdec = sb("kdec", [64, 8, 64], F32)
    vbm = sb("vbm", [64, 8, 64], F32)
    um = sb("um", [64, 8, 64], F32)
    wTm = sb("wTm", [64, 8, 64], F32)
    Sst = sb("Sst", [64, 64], F32)
    vnew = sb("vnew", [64, 64], F32)
    otm = sb("otm", [64, 8, 64], BF16)

    def bc3(ap2, n):
        return ap2.unsqueeze(2).to_broadcast([ap2.shape[0], ap2.shape[1], n])

    def bcmid(ap2, n):
        return ap2.unsqueeze(1).to_broadcast([ap2.shape[0], n, ap2.shape[1]])

    def macro(m):
        b = m // MPB
        qi = m % MPB
        pb = 64 * b
        PB = slice(pb, pb + 64)
        for j in range(4):
            t = 4 * m + j
            X = xt[t % 2]
            XN = xn[t % 2]
            dma("sp", lambda e, X=X, t=t: e.dma_start(out=X[:, :], in_=io["x_all"][t * 128:(t + 1) * 128, :]),
                writes=[X.b])
            op("act", lambda e, X=X: e.activation(out=junk[:, :], in_=X[:, :], func=AF.Square, accum_out=ssq[:, 0:1]),
               reads=[X.b], writes=[junk.b, ssq.b])
            op("act", lambda e: e.activation(out=rstd[:, :], in_=ssq[:, :], func=AF.Sqrt, scale=1.0 / D,
                                             bias=CF("eps")), reads=[ssq.b, cf.b], writes=[rstd.b])
            op("dve", lambda e: e.reciprocal(out=rstd[:, :], in_=rstd[:, :]), reads=[rstd.b], writes=[rstd.b])
            op("dve", lambda e, X=X, XN=XN: e.tensor_scalar(out=XN[:, :], in0=X[:, :], scalar1=rstd[:, 0:1],
                                                            scalar2=None, op0=ALU.mult),
               reads=[X.b, rstd.b], writes=[XN.b])
            tp = bank()
            tpv = tp[:, :].bitcast(BF16)
            for c in range(8):
                op("pe", lambda e, c=c, XN=XN, tpv=tpv: e.transpose(
                    out=tpv[:, c * 128:(c + 1) * 128], in_=XN[:, c * 128:(c + 1) * 128], identity=CB("ident")),
                   reads=[XN.b, cb.b], writes=[tp.b])
            for c in range(8):
                op("dve", lambda e, c=c, j=j, tpv=tpv, b=b: e.tensor_scalar(
                    out=hT[:, c, j * 128:(j + 1) * 128], in0=tpv[:, c * 128:(c + 1) * 128],
                    scalar1=G1[:, c, b:b + 1], scalar2=modT[:, c, b:b + 1], op0=ALU.mult, op1=ALU.add),
                   reads=[tp.b, G1.b, modT.b], writes=[hT.b])
        if STAGE < 3:
            return
        pv = bank()
        for j in range(4):
            for c in range(8):
                op("pe", lambda e, j=j, c=c, pv=pv: e.matmul(
                    pv[:, j * 128:j * 128 + 66], lhsT=hT[:, c, j * 128:(j + 1) * 128], rhs=Wv66[:, c, :],
                    start=(c == 0), stop=(c == 7)), reads=[hT.b, Wv66.b], writes=[pv.b])
        pv3 = pv[:, :].rearrange("p (j n) -> p j n", n=128)
        op("act", lambda e, pv3=pv3, m=m: e.copy(out=Vt[:, 4 * m:4 * m + 4, :], in_=pv3[:, :, 0:64]),
           reads=[pv.b], writes=[Vtb[m]])
        op("act", lambda e, pv3=pv3: e.activation(out=tmpab[:, :], in_=pv3[:, :, 64], func=AF.Exp, bias=hv[:, 3:4]),
           reads=[pv.b, hv.b], writes=[tmpab.b])
        op("act", lambda e: e.activation(out=tmpab[:, :], in_=tmpab[:, :], func=AF.Ln, bias=CF("one")),
           reads=[tmpab.b, cf.b], writes=[tmpab.b])
        op("dve", lambda e: e.tensor_scalar(out=gb[:, 0, :], in0=tmpab[:, :], scalar1=negA[:, 0:1], scalar2=None,
                                            op0=ALU.mult), reads=[tmpab.b, negA.b], writes=[gb.b])
        op("act", lambda e, pv3=pv3: e.activation(out=gb[:, 1, :], in_=pv3[:, :, 65], func=AF.Sigmoid),
           reads=[pv.b], writes=[gb.b])
        for W_, wcol, dst, dstb in ((Wqq, None, qT, qT.b), (Wkk, 1, KT, KTb[m])):
            pq = bank()
            for c in range(8):
                op("pe", lambda e, c=c, pq=pq, W_=W_: e.matmul(pq[:, :], lhsT=W_[:, c, :], rhs=hT[:, c, :],
                                                              start=(c == 0), stop=(c == 7)),
                   reads=[hT.b, W_.b], writes=[pq.b])
            op("act", lambda e, pq=pq: e.activation(out=sqb[:, :], in_=pq[:, :], func=AF.Square),
               reads=[pq.b], writes=[sqb.b])
            pss = bank()
            op("pe", lambda e, pss=pss: e.matmul(pss[:, :], lhsT=CB("blockones"), rhs=sqb[:, :], start=True, stop=True),
               reads=[sqb.b, cb.b], writes=[pss.b])
            op("act", lambda e, pss=pss: e.activation(out=rt[:, :], in_=pss[:, :], func=AF.Sqrt, scale=1.0 / 64,
                                                      bias=CF("eps")), reads=[pss.b, cf.b], writes=[rt.b])
            op("dve", lambda e: e.reciprocal(out=rt[:, :], in_=rt[:, :]), reads=[rt.b], writes=[rt.b])
            if wcol is None:
                sc_ap = wq8[PB, 0:1]
                scb = wq8.b
                out_ap = qT[PB, :]
            else:
                sc_ap = hv[PB, 1:2]
                scb = hv.b
                out_ap = KT[PB, qi * NQ:(qi + 1) * NQ]
            op("dve", lambda e, pq=pq, sc_ap=sc_ap, out_ap=out_ap: e.scalar_tensor_tensor(
                out=out_ap, in0=pq[PB, :], scalar=sc_ap, in1=rt[PB, :], op0=ALU.mult, op1=ALU.mult),
               reads=[pq.b, scb, rt.b], writes=[dstb])
        if STAGE < 4:
            return
        first = True
        kbs = list(range(4 * qi + 3, -1, -1))
        for ii, kb in enumerate(kbs):
            r = kb - 4 * qi
            last = (ii == len(kbs) - 1)
            mk = kb // 4 + b * MPB
            LP = Lp[ii % 2]
            AT = At[ii % 2]
            ktap = KT[PB, kb * 128:(kb + 1) * 128]
            Z = bank()
            op("pe", lambda e, Z=Z, ktap=ktap: e.matmul(Z[:, :], lhsT=ktap, rhs=qT[PB, :], start=True, stop=True),
               reads=[KTb[mk], qT.b], writes=[Z.b])
            op("act", lambda e, Z=Z: e.activation(out=Esb[:, :], in_=Z[:, :], func=AF.Exp), reads=[Z.b], writes=[Esb.b])
            op("act", lambda e, LP=LP: e.activation(out=LP[:, :], in_=Esb[:, :], func=AF.Ln, bias=CF("one")),
               reads=[Esb.b, cf.b], writes=[LP.b])
            if r >= 0:
                op("pool", lambda e, LP=LP, r=r: e.tensor_tensor(out=LP[:, :], in0=LP[:, :], in1=CB(f"dmask{r}"),
                                                                 op=ALU.mult), reads=[LP.b, cb.b], writes=[LP.b])
            P = bank()
            op("pe", lambda e, P=P, ktap=ktap: e.matmul(P[:, :], lhsT=ktap, rhs=qT[PB, :], start=True, stop=False),
               reads=[KTb[mk], qT.b], writes=[P.b])
            op("pe", lambda e, P=P, LP=LP, first=first: e.matmul(P[:, :], lhsT=CB("negtri"), rhs=LP[:, :],
                                                                 start=False, stop=first),
               reads=[LP.b, cb.b], writes=[P.b])
            if not first:
                op("pe", lambda e, P=P: e.matmul(P[:, :], lhsT=CB("negones"), rhs=Lacc[:, :], start=False, stop=True),
                   reads=[Lacc.b, cb.b], writes=[P.b])
            op("act", lambda e, P=P, AT=AT: e.activation(out=AT[:, :], in_=P[:, :], func=AF.Exp),
               reads=[P.b], writes=[AT.b])
            if r >= 0:
                op("pool", lambda e, AT=AT, r=r: e.tensor_tensor(out=AT[:, :], in0=AT[:, :], in1=CB(f"dmask{r}"),
                                                                 op=ALU.mult), reads=[AT.b, cb.b], writes=[AT.b])
            vt_idx = b * NKB + kb
            op("pe", lambda e, AT=AT, vt_idx=vt_idx, first=first, last=last: e.matmul(
                OT[0:64, :], lhsT=Vt[:, vt_idx, :], rhs=AT[:, :], start=first, stop=last),
               reads=[Vtb[mk], AT.b], writes=[OT.b])
            if first:
                op("pool", lambda e, LP=LP: e.tensor_copy(out=Lacc[:, :], in_=LP[:, :]), reads=[LP.b], writes=[Lacc.b])
            elif not last:
                op("pool", lambda e, LP=LP: e.tensor_tensor(out=Lacc[:, :], in0=Lacc[:, :], in1=LP[:, :], op=ALU.add),
                   reads=[LP.b, Lacc.b], writes=[Lacc.b])
            first = False
        op("act", lambda e: e.copy(out=ysb[:, :], in_=OT[0:64, :]), reads=[OT.b], writes=[ysb.b])
        for (dst_ap, src_ap) in io["ysb_dst"](m, ysb):
            dma("sp", lambda e, dst_ap=dst_ap, src_ap=src_ap: e.dma_start(out=dst_ap, in_=src_ap),
                reads=[ysb.b], writes=[io["ysb_out_b"]])

        if STAGE < 5:
            return
        if qi == 0:
            op("dve", lambda e: e.memset(cq[:, 0:3], 0.0), writes=[cq.b])
            op("dve", lambda e: e.memset(cv[:, 0:3], 0.0), writes=[cv.b])
            op("dve", lambda e: e.memset(Sst[:, :], 0.0), writes=[Sst.b])
        pg = bank()
        for c in range(8):
            op("pe", lambda e, c=c, pg=pg: e.matmul(pg[:, :], lhsT=wh[:, c, 192:320], rhs=hT[:, c, :],
                                                    start=(c == 0), stop=(c == 7)), reads=[hT.b, wh.b], writes=[pg.b])
        op("act", lambda e, pg=pg: e.copy(out=cq[:, 3:NQ + 3], in_=pg[:, :]), reads=[pg.b], writes=[cq.b])
        pgv = bank()
        for c in range(8):
            op("pe", lambda e, c=c, pgv=pgv: e.matmul(pgv[0:64, :], lhsT=wh[:, c, 320:384], rhs=hT[:, c, :],
                                                      start=(c == 0), stop=(c == 7)), reads=[hT.b, wh.b], writes=[pgv.b])
        op("act", lambda e, pgv=pgv: e.copy(out=cv[:, 3:NQ + 3], in_=pgv[0:64, :]), reads=[pgv.b], writes=[cv.b])
        for (CX, ACC, HV, off, rows) in ((cq, acc, hv, 4, slice(0, 128)), (cv, accv, hv2, 0, slice(0, 64))):
            op("dve", lambda e, CX=CX, ACC=ACC, HV=HV, off=off, rows=rows: e.tensor_scalar(
                out=ACC[:, :], in0=CX[:, 0:NQ], scalar1=HV[rows, off:off + 1], scalar2=None, op0=ALU.mult),
               reads=[CX.b, HV.b], writes=[ACC.b])
            for jj in range(1, 4):
                op("dve", lambda e, CX=CX, ACC=ACC, HV=HV, off=off, rows=rows, jj=jj: e.scalar_tensor_tensor(
                    out=ACC[:, :], in0=CX[:, jj:jj + NQ], scalar=HV[rows, off + jj:off + jj + 1], in1=ACC[:, :],
                    op0=ALU.mult, op1=ALU.add), reads=[CX.b, HV.b, ACC.b], writes=[ACC.b])
            op("dve", lambda e, CX=CX: e.tensor_copy(out=CX[:, 0:3], in_=CX[:, NQ:NQ + 3]), reads=[CX.b], writes=[CX.b])
        op("act", lambda e: e.activation(out=sq[:, :], in_=acc[:, :], func=AF.Silu), reads=[acc.b], writes=[sq.b])
        op("act", lambda e: e.activation(out=vT0[:, :], in_=accv[:, :], func=AF.Silu), reads=[accv.b], writes=[vT0.b])
        op("dve", lambda e: e.tensor_tensor(out=sqq[:, :], in0=sq[:, :], in1=sq[:, :], op=ALU.mult),
           reads=[sq.b], writes=[sqq.b])
        if STAGE < 6:
            return
        pn = bank()
        op("pe", lambda e, pn=pn: e.matmul(pn[:, :], lhsT=CF("blockones"), rhs=sqq[:, :], start=True, stop=True),
           reads=[sqq.b, cf.b], writes=[pn.b])
        op("act", lambda e, pn=pn: e.activation(out=sqq[:, :], in_=pn[:, :], func=AF.Sqrt, bias=CF("eps")),
           reads=[pn.b, cf.b], writes=[sqq.b])
        op("dve", lambda e: e.reciprocal(out=sqq[:, :], in_=sqq[:, :]), reads=[sqq.b], writes=[sqq.b])
        op("dve", lambda e: e.scalar_tensor_tensor(out=qkn[:, :], in0=sq[:, :], scalar=CF("l2s"), in1=sqq[:, :],
                                                   op0=ALU.mult, op1=ALU.mult), reads=[sq.b, sqq.b, cf.b], writes=[qkn.b])
        pk = bank()
        op("pe", lambda e, pk=pk: e.matmul(pk[0:64, :], lhsT=CF("shift"), rhs=qkn[:, :], start=True, stop=True),
           reads=[qkn.b, cf.b], writes=[pk.b])
        op("act", lambda e, pk=pk: e.copy(out=kT0[:, :], in_=pk[0:64, :]), reads=[pk.b], writes=[kT0.b])
        if STAGE < 7:
            return
        pgs = bank()
        op("pe", lambda e, pgs=pgs: e.matmul(pgs[0:64, 0:8], lhsT=CF("ident")[:, 0:64],
                                             rhs=gb[:, :, :].rearrange("p x j -> p (x j)"), start=True, stop=True),
           reads=[gb.b, cf.b], writes=[pgs.b])
        op("pe", lambda e, pgs=pgs: e.matmul(pgs[0:64, 8:16], lhsT=CF("shift"),
                                             rhs=gb[:, :, :].rearrange("p x j -> p (x j)"), start=True, stop=True),
           reads=[gb.b, cf.b], writes=[pgs.b])
        op("dve", lambda e, pgs=pgs: e.tensor_copy(out=gsc[:, :, :].rearrange("p x (j h) -> p h x j", h=2),
                                                   in_=pgs[0:64, 0:16].rearrange("p (h x j) -> p h x j", h=2, x=2)),
           reads=[pgs.b], writes=[gsc.b])
        pG = bank()
        op("pe", lambda e, pG=pG: e.matmul(pG[0:64, 0:8], lhsT=CF("triu", slice(0, 64)), rhs=gsc[:, 0, :], start=True, stop=True),
           reads=[gsc.b, cf.b], writes=[pG.b])
        op("pe", lambda e, pG=pG: e.matmul(pG[0:64, 8:16], lhsT=CF("ones", slice(0, 64))[:, 0:64], rhs=gsc[:, 0, :],
                                           start=True, stop=True), reads=[gsc.b, cf.b], writes=[pG.b])
        op("dve", lambda e, pG=pG: e.tensor_copy(out=Gcol[:, :], in_=pG[0:64, 0:8]), reads=[pG.b], writes=[Gcol.b])
        op("dve", lambda e, pG=pG: e.tensor_copy(out=GLs[:, :], in_=pG[0:64, 8:16]), reads=[pG.b], writes=[GLs.b])
        op("act", lambda e: e.activation(out=expG[:, :], in_=Gcol[:, :], func=AF.Exp), reads=[Gcol.b], writes=[expG.b])
        op("act", lambda e: e.activation(out=cd[:, :], in_=GLs[:, :], func=AF.Exp), reads=[GLs.b], writes=[cd.b])
        op("dve", lambda e: e.tensor_tensor(out=kdecs[:, :], in0=GLs[:, :], in1=Gcol[:, :], op=ALU.subtract),
           reads=[GLs.b, Gcol.b], writes=[kdecs.b])
        op("act", lambda e: e.activation(out=kdecs[:, :], in_=kdecs[:, :], func=AF.Exp), reads=[kdecs.b], writes=[kdecs.b])
        op("dve", lambda e: e.tensor_tensor(out=bexpG[:, :], in0=expG[:, :], in1=gsc[:, 1, :], op=ALU.mult),
           reads=[expG.b, gsc.b], writes=[bexpG.b])
        if STAGE < 8:
            return
        op("dve", lambda e: e.tensor_tensor(out=diagG[:, :, :], in0=bcmid(CF("id64", slice(0, 64)), 8),
                                            in1=bc3(Gcol[:, :], 64), op=ALU.mult),
           reads=[cf.b, Gcol.b], writes=[diagG.b])
        pR = bank()
        op("pe", lambda e, pR=pR: e.matmul(pR[0:64, :], lhsT=CF("ones", slice(0, 64))[:, 0:64],
                                           rhs=diagG[:, :, :].rearrange("p c j -> p (c j)"), start=True, stop=True),
           reads=[diagG.b, cf.b], writes=[pR.b])
        if STAGE < 8.2:
            return
        op("dve", lambda e, pR=pR: e.tensor_copy(out=Rsb[:, :], in_=pR[0:64, :]), reads=[pR.b], writes=[Rsb.b])
        op("dve", lambda e: e.tensor_tensor(out=negD[:, :, :], in0=Rsb[:, :].rearrange("p (c j) -> p c j", j=64),
                                            in1=bc3(Gcol[:, :], 64), op=ALU.subtract),
           reads=[Rsb.b, Gcol.b], writes=[negD.b])
        op("act", lambda e: e.activation(out=eR[:, :], in_=Rsb[:, :], func=AF.Exp), reads=[Rsb.b], writes=[eR.b])
        if STAGE < 8.4:
            return
        op("dve", lambda e: e.tensor_scalar(out=e2[:, :, :], in0=negD[:, :, :], scalar1=0.0, scalar2=None, op0=ALU.min),
           reads=[negD.b], writes=[e2.b])
        op("dve", lambda e: e.tensor_scalar(out=e1[:, :, :], in0=negD[:, :, :], scalar1=-1.0, scalar2=0.0, op0=ALU.mult,
                                            op1=ALU.min), reads=[negD.b], writes=[e1.b])
        if STAGE < 8.6:
            return
        op("act", lambda e: e.activation(out=e2[:, :, :], in_=e2[:, :, :], func=AF.Exp), reads=[e2.b], writes=[e2.b])
        op("act", lambda e: e.activation(out=e1[:, :, :], in_=e1[:, :, :], func=AF.Exp), reads=[e1.b], writes=[e1.b])
        if STAGE < 8.8:
            return
        op("dve", lambda e: e.tensor_tensor(out=e2[:, :, :], in0=e2[:, :, :], in1=bcmid(CF("ui", slice(0, 64)), 8), op=ALU.mult),
           reads=[e2.b, cf.b], writes=[e2.b])
        op("dve", lambda e: e.tensor_tensor(out=e1[:, :, :], in0=e1[:, :, :], in1=bcmid(CF("ls", slice(0, 64)), 8), op=ALU.mult),
           reads=[e1.b, cf.b], writes=[e1.b])
        op("dve", lambda e: e.tensor_tensor(out=e1[:, :, :], in0=e1[:, :, :], in1=bc3(gsc[:, 1, :], 64), op=ALU.mult),
           reads=[e1.b, gsc.b], writes=[e1.b])
        op("dve", lambda e: e.tensor_tensor(out=qdT[:, :], in0=qkn[0:64, :], in1=eR[:, :], op=ALU.mult),
           reads=[qkn.b, eR.b], writes=[qdT.b])
        if STAGE < 9:
            return
        pkk = bank()
        pkq = bank()
        for ch in range(8):
            cs = slice(ch * 64, (ch + 1) * 64)
            op("pe", lambda e, cs=cs, pkk=pkk: e.matmul(pkk[0:64, cs], lhsT=kT0[:, cs], rhs=kT0[:, cs], start=True, stop=True),
               reads=[kT0.b], writes=[pkk.b])
            op("pe", lambda e, cs=cs, pkq=pkq: e.matmul(pkq[0:64, cs], lhsT=kT0[:, cs], rhs=qkn[0:64, cs], start=True, stop=True),
               reads=[kT0.b, qkn.b], writes=[pkq.b])
        A0, B0, X0 = Ak[0], Bk[0], Xk[0]
        op("dve", lambda e, pkk=pkk, A0=A0: e.tensor_tensor(out=A0[:, :, :].rearrange("p c j -> p (c j)"), in0=pkk[0:64, :],
                                                           in1=e1[:, :, :].rearrange("p c j -> p (c j)"), op=ALU.mult),
           reads=[pkk.b, e1.b], writes=[A0.b])
        op("dve", lambda e, pkq=pkq: e.tensor_tensor(out=qkT[:, :, :].rearrange("p c j -> p (c j)"), in0=pkq[0:64, :],
                                                    in1=e2[:, :, :].rearrange("p c j -> p (c j)"), op=ALU.mult),
           reads=[pkq.b, e2.b], writes=[qkT.b])
        if STAGE < 10:
            return
        pB = bank()
        for ch in range(8):
            cs = slice(ch * 64, (ch + 1) * 64)
            op("pe", lambda e, ch=ch, cs=cs, pB=pB, A0=A0: e.transpose(out=pB[0:64, cs], in_=A0[:, ch, :],
                                                                      identity=CF("ident", slice(0, 64))[:, 0:64]),
               reads=[A0.b, cf.b], writes=[pB.b])
        op("act", lambda e, pB=pB, B0=B0: e.copy(out=B0[:, :, :].rearrange("p c j -> p (c j)"), in_=pB[0:64, :]),
           reads=[pB.b], writes=[B0.b])
        op("dve", lambda e, B0=B0, X0=X0: e.tensor_tensor(out=X0[:, :, :], in0=bcmid(CF("id64", slice(0, 64)), 8),
                                                         in1=B0[:, :, :], op=ALU.subtract),
           reads=[B0.b, cf.b], writes=[X0.b])
        if STAGE < 11:
            return
        for k in range(5):
            Aa, Ab = Ak[k % 2], Ak[(k + 1) % 2]
            Ba, Bb = Bk[k % 2], Bk[(k + 1) % 2]
            Xa, Xb = Xk[k % 2], Xk[(k + 1) % 2]
            pA = bank()
            for ch in range(8):
                cs = slice(ch * 64, (ch + 1) * 64)
                op("pe", lambda e, ch=ch, cs=cs, pA=pA, Aa=Aa, Ba=Ba: e.matmul(pA[0:64, cs], lhsT=Ba[:, ch, :], rhs=Aa[:, ch, :],
                                                                              start=True, stop=True),
                   reads=[Aa.b, Ba.b], writes=[pA.b])
            op("act", lambda e, pA=pA, Ab=Ab: e.copy(out=Ab[:, :, :].rearrange("p c j -> p (c j)"), in_=pA[0:64, :]),
               reads=[pA.b], writes=[Ab.b])
            if k < 4:
                pBn = bank()
                for ch in range(8):
                    cs = slice(ch * 64, (ch + 1) * 64)
                    op("pe", lambda e, ch=ch, cs=cs, pBn=pBn, Aa=Aa, Ba=Ba: e.matmul(pBn[0:64, cs], lhsT=Aa[:, ch, :],
                                                                                    rhs=Ba[:, ch, :], start=True, stop=True),
                       reads=[Aa.b, Ba.b], writes=[pBn.b])
                op("act", lambda e, pBn=pBn, Bb=Bb: e.copy(out=Bb[:, :, :].rearrange("p c j -> p (c j)"), in_=pBn[0:64, :]),
                   reads=[pBn.b], writes=[Bb.b])
            pX = bank()
            for ch in range(8):
                cs = slice(ch * 64, (ch + 1) * 64)
                op("pe", lambda e, ch=ch, cs=cs, pX=pX, Ab=Ab, Xa=Xa: e.matmul(pX[0:64, cs], lhsT=Ab[:, ch, :], rhs=Xa[:, ch, :],
                                                                              start=True, stop=True),
                   reads=[Ab.b, Xa.b], writes=[pX.b])
            op("dve", lambda e, pX=pX, Xa=Xa, Xb=Xb: e.tensor_tensor(out=Xb[:, :, :].rearrange("p c j -> p (c j)"),
                                                                    in0=Xa[:, :, :].rearrange("p c j -> p (c j)"),
                                                                    in1=pX[0:64, :], op=ALU.add),
               reads=[pX.b, Xa.b], writes=[Xb.b])
        XF = Xk[1]
        if STAGE < 12:
            return
        pT1 = bank()
        pT2 = bank()
        for ch in range(8):
            pt = pT1 if ch < 4 else pT2
            o = (ch % 4) * 128
            op("pe", lambda e, ch=ch, pt=pt, o=o: e.transpose(out=pt[0:64, o:o + 128], in_=qkn[:, ch * 64:(ch + 1) * 64],
                                                             identity=CF("ident")), reads=[qkn.b, cf.b], writes=[pt.b])
        for half, pt in ((0, pT1), (1, pT2)):
            ktm = pt[0:64, :].rearrange("p (c x) -> p c x", x=128)[:, :, 64:128]
            hs = slice(half * 4, half * 4 + 4)
            op("dve", lambda e, ktm=ktm, hs=hs: e.tensor_tensor(out=kbm[:, hs, :], in0=ktm, in1=bc3(bexpG[:, hs], 64), op=ALU.mult),
               reads=[pt.b, bexpG.b], writes=[kbm.b])
            op("dve", lambda e, ktm=ktm, hs=hs: e.tensor_tensor(out=kdec[:, hs, :], in0=ktm, in1=bc3(kdecs[:, hs], 64), op=ALU.mult),
               reads=[pt.b, kdecs.b], writes=[kdec.b])
        pTv = bank()
        for ch in range(8):
            cs = slice(ch * 64, (ch + 1) * 64)
            op("pe", lambda e, cs=cs, pTv=pTv: e.transpose(out=pTv[0:64, cs], in_=vT0[:, cs],
                                                          identity=CF("ident", slice(0, 64))[:, 0:64]),
               reads=[vT0.b, cf.b], writes=[pTv.b])
        op("dve", lambda e, pTv=pTv: e.tensor_tensor(out=vbm[:, :, :], in0=pTv[0:64, :].rearrange("p (c x) -> p c x", x=64),
                                                    in1=bc3(gsc[:, 1, :], 64), op=ALU.mult),
           reads=[pTv.b, gsc.b], writes=[vbm.b])
        if STAGE < 13:
            return
        pU = bank()
        pW = bank()
        for ch in range(8):
            cs = slice(ch * 64, (ch + 1) * 64)
            op("pe", lambda e, ch=ch, cs=cs, pU=pU: e.matmul(pU[0:64, cs], lhsT=XF[:, ch, :], rhs=vbm[:, ch, :], start=True, stop=True),
               reads=[XF.b, vbm.b], writes=[pU.b])
            op("pe", lambda e, ch=ch, cs=cs, pW=pW: e.matmul(pW[0:64, cs], lhsT=kbm[:, ch, :], rhs=XF[:, ch, :], start=True, stop=True),
               reads=[XF.b, kbm.b], writes=[pW.b])
        op("act", lambda e, pU=pU: e.copy(out=um[:, :, :].rearrange("p c j -> p (c j)"), in_=pU[0:64, :]), reads=[pU.b], writes=[um.b])
        op("act", lambda e, pW=pW: e.copy(out=wTm[:, :, :].rearrange("p c j -> p (c j)"), in_=pW[0:64, :]), reads=[pW.b], writes=[wTm.b])
        if STAGE < 14:
            return
        for ch in range(8):
            cs = slice(ch * 64, (ch + 1) * 64)
            p1 = bank()
            op("pe", lambda e, ch=ch, p1=p1: e.matmul(p1[0:64, 0:64], lhsT=wTm[:, ch, :], rhs=Sst[:, :], start=True, stop=True),
               reads=[wTm.b, Sst.b], writes=[p1.b])
            op("dve", lambda e, ch=ch, p1=p1: e.tensor_tensor(out=vnew[:, :], in0=um[:, ch, :], in1=p1[0:64, 0:64], op=ALU.subtract),
               reads=[um.b, p1.b], writes=[vnew.b])
            p2 = bank()
            op("pe", lambda e, cs=cs, p2=p2: e.matmul(p2[0:64, 0:64], lhsT=qdT[:, cs], rhs=Sst[:, :], start=True, stop=False),
               reads=[qdT.b, Sst.b], writes=[p2.b])
            op("pe", lambda e, ch=ch, p2=p2: e.matmul(p2[0:64, 0:64], lhsT=qkT[:, ch, :], rhs=vnew[:, :], start=False, stop=True),
               reads=[qkT.b, vnew.b], writes=[p2.b])
            op("act", lambda e, ch=ch, p2=p2: e.copy(out=otm[:, ch, :], in_=p2[0:64, 0:64]), reads=[p2.b], writes=[otm.b])
            p3 = bank()
            op("pe", lambda e, ch=ch, p3=p3: e.matmul(p3[0:64, 0:64], lhsT=kdec[:, ch, :], rhs=vnew[:, :], start=True, stop=True),
               reads=[kdec.b, vnew.b], writes=[p3.b])
            op("dve", lambda e, ch=ch, p3=p3: e.scalar_tensor_tensor(out=Sst[:, :], in0=Sst[:, :], scalar=cd[:, ch:ch + 1],
                                                                    in1=p3[0:64, 0:64], op0=ALU.mult, op1=ALU.add),
               reads=[Sst.b, cd.b, p3.b], writes=[Sst.b])
        for (dst_ap, src_ap) in io["ogdn_dst"](m, otm):
            dma("sp", lambda e, dst_ap=dst_ap, src_ap=src_ap: e.dma_start(out=dst_ap, in_=src_ap),
                reads=[otm.b], writes=[io["ogdn_out_b"]])

    for m in range(NM):
        macro(m)
    return dict(cf=cf, cb=cb, CF=CF, CB=CB, modT=modT, G1=G1, scT=scT, banks=banks)


def build_B(nc, S, sch, stack, io, A):
    TT = 2 * S
    TOK = TT // 8
    NT = TOK // 128
    op = sch.op
    dma = sch.dma
    modT, G1, scT, banks = (A[k] for k in ("modT", "G1", "scT", "banks"))

    def sb(name, shape, dt):
        return T(stack.enter_context(nc.sbuf_tensor("b_" + name, shape, dt)), name)

    rot = [0]

    def bank():
        b = banks[rot[0] % 8]
        rot[0] += 1
        return b

    def bc3(ap2, n):
        return ap2.unsqueeze(2).to_broadcast([ap2.shape[0], ap2.shape[1], n])

    def bcmid(ap2, n):
        return ap2.unsqueeze(1).to_broadcast([ap2.shape[0], n, ap2.shape[1]])

    identb = sb("identb", [128, 128], BF16)
    smallc = sb("smallc", [128, 128 + 1 + 16], F32)
    cf = smallc
    cb = identb
    o_id, _ = CONST_OFF["ident"]
    o_eps, _ = CONST_OFF["eps"]
    o_io, _ = CONST_OFF["iota16"]
    dma("sp", lambda e: e.dma_start(out=smallc[:, 0:128], in_=io["consts"][:, o_id:o_id + 128]), writes=[smallc.b])
    dma("sp", lambda e: e.dma_start(out=smallc[:, 128:129], in_=io["consts"][:, o_eps:o_eps + 1], allow_slow_non_contiguous=True), writes=[smallc.b])
    dma("sp", lambda e: e.dma_start(out=smallc[:, 129:145], in_=io["consts"][:, o_io:o_io + 16]), writes=[smallc.b])
    op("dve", lambda e: e.tensor_copy(out=identb[:, :], in_=smallc[:, 0:128]), reads=[smallc.b], writes=[identb.b])

    def CF(k):
        return {"eps": smallc[:, 128:129], "iota16": smallc[:, 129:145]}[k]

    def CB(k):
        assert k == "ident"
        return identb[:, :]

    import contextlib as _cl
    setup = _cl.ExitStack()

    def sbs(name, shape, dt):
        return T(setup.enter_context(nc.sbuf_tensor("b_" + name, shape, dt)), name)

    Wzg = sb("Wzg", [128, 8, 2560], BF16)
    Wsb = sb("Wsb", [128, 4, 1024], BF16)
    Wgd = sb("Wgd", [128, 4, 1024], BF16)
    Wo = sb("Wo", [128, 8, 1024], BF16)
    Wq = sb("Wq", [128, 8, 2048], BF16)
    skT = sb("skT", [128, 16, 128], BF16)
    gate_b = [sb(f"gateb{i}", [128, 1024], BF16) for i in range(2)]
    n2w = sb("n2w", [128, 8], F32)
    G2 = sb("G2", [128, 8], F32)
    onw = sb("onw", [128, 64], F32)
    wst = sbs("wst", [128, 8, 512], F32)
    screp = sbs("screp", [128, 8, 128], F32)
    brow = sbs("brow", [128, 1024], F32)

    def load_w(src, dst, kch, c0, ncols, dcol0):
        dma("sp", lambda e: e.dma_start(out=wst[:, 0:kch, 0:ncols],
                                        in_=src[:, c0:c0 + ncols].rearrange("(c p) n -> p c n", p=128)), writes=[wst.b])
        op("act", lambda e: e.copy(out=dst[:, 0:kch, dcol0:dcol0 + ncols], in_=wst[:, 0:kch, 0:ncols]),
           reads=[wst.b], writes=[dst.b])

    for g in range(5):
        load_w(io["w_zg"], Wzg, 8, g * 512, 512, g * 512)
    for g in range(2):
        load_w(io["w_psb"], Wsb, 4, g * 512, 512, g * 512)
        load_w(io["w_pgdn"], Wgd, 4, g * 512, 512, g * 512)
        load_w(io["w_o"], Wo, 8, g * 512, 512, g * 512)
    for g in range(4):
        load_w(io["peer_wq"], Wq, 8, g * 512, 512, g * 512)
    skv = wst[:, 0:4, :].rearrange("p a (c k) -> p (a c) k", k=128)
    dma("sp", lambda e: e.dma_start(out=skv, in_=io["skT"][:, :, :]), writes=[wst.b])
    op("act", lambda e: e.copy(out=skT[:, :, :], in_=skv), reads=[wst.b], writes=[skT.b])
    op("dve", lambda e: e.tensor_copy(out=screp[:, :, :], in_=scT[:, :, 2:3].to_broadcast([128, 8, 128])),
       reads=[scT.b], writes=[screp.b])
    for gi, c0 in ((0, 2048), (1, 5120)):
        dma("sp", lambda e, c0=c0: e.dma_start(out=brow[:, :], in_=io["b_ada_row"][0:1, c0:c0 + 1024].to_broadcast([128, 1024])),
            writes=[brow.b])
        for hf in range(2):
            dma("sp", lambda e, c0=c0, hf=hf: e.dma_start(out=wst[:, :, :],
                                                         in_=io["w_ada"][:, c0 + hf * 512:c0 + (hf + 1) * 512].rearrange("(c p) n -> p c n", p=128)),
                writes=[wst.b])
            pb_ = bank()
            for kc in range(8):
                op("pe", lambda e, kc=kc, hf=hf, pb_=pb_: e.matmul(pb_[:, :], lhsT=screp[:, kc, :], rhs=wst[:, kc, :],
                                                                  start=(kc == 0), stop=(kc == 7)), reads=[screp.b, wst.b], writes=[pb_.b])
            op("dve", lambda e, hf=hf, pb_=pb_, gi=gi: e.tensor_tensor(out=gate_b[gi][:, hf * 512:(hf + 1) * 512], in0=pb_[:, :],
                                                                      in1=brow[:, hf * 512:(hf + 1) * 512], op=ALU.add),
               reads=[pb_.b, brow.b], writes=[gate_b[gi].b])
    dma("sp", lambda e: e.dma_start(out=n2w[:, :], in_=io["n2wT"][:, :]), writes=[n2w.b])
    dma("sp", lambda e: e.dma_start(out=onw[:, :], in_=io["onw"][:, :]), writes=[onw.b])
    op("dve", lambda e: e.scalar_tensor_tensor(out=G2[:, :], in0=modT[:, 32:40, 2], scalar=1.0, in1=n2w[:, :], op0=ALU.add, op1=ALU.mult),
       reads=[modT.b, n2w.b], writes=[G2.b])

    sch.full_barrier()
    setup.close()
    dma("pool", lambda e: e.dma_start(
        out=io["ysb_own"][:, :, :],
        in_=io["ysb_g"].rearrange("(s j) d t -> j s d t", j=8)[bass.ds(sch.E["pool"].core, 1), :, :, :].rearrange("a s d t -> (a s) d t")),
        reads=[io["xchg_b"]], writes=[io["own_b"]])
    dma("pool", lambda e: e.dma_start(
        out=io["ogdn_own"][:, :, :],
        in_=io["ogdn_g"].rearrange("(s j) t e -> j s t e", j=8)[bass.ds(sch.E["pool"].core, 1), :, :, :].rearrange("a s t e -> (a s) t e")),
        reads=[io["xchg_b"]], writes=[io["own_b"]])
    ysT = sb("ysT", [128, 4, 128], BF16)
    og = sb("og", [128, 8, 64], BF16)

    xt = sb("xt", [128, 1024], F32)
    xn = sb("xn", [128, 1024], BF16)
    ssq = sb("ssq", [128, 1], F32)
    rstd = sb("rstd", [128, 1], F32)
    hT = sb("hT", [128, 8, 128], BF16)
    oss = sb("oss", [128, 8], F32)
    onb = sb("onb", [128, 512], BF16)
    ygT = sb("ygT", [128, 4, 128], BF16)
    mg = sb("mg", [128, 1024], BF16)
    mT = sb("mT", [128, 8, 128], BF16)
    x1 = sb("x1", [128, 1024], F32)
    h2T = sb("h2T", [128, 8, 128], BF16)
    h2 = sb("h2", [128, 1024], F32)
    qTt = sb("qTt", [128, 16, 128], BF16)
    sc = sb("sc", [128, 16, 128], F32)

    class _V:
        def __init__(self, base, view):
            self.b = base.b
            self.v = view

        def __getitem__(self, k):
            return self.v[k]

    scf = sc[:, :, :].rearrange("p c k -> p (c k)")
    osq = _V(sc, scf[:, 0:512].rearrange("p (h e) -> p h e", e=64))
    on = _V(sc, scf[:, 512:1024].rearrange("p (h e) -> p h e", e=64))
    sz = _V(sc, scf[:, 1024:1536])
    gts = _V(qTt, qTt[:, :, :].rearrange("p c k -> p (c k)"))
    sc2 = sb("sc2", [128, 256], F32)
    v16 = sb("v16", [128, 16, 16], F32)
    i16 = sb("i16", [128, 16, 16], U32)
    i16f = sb("i16f", [128, 16, 16], F32)
    cand = sb("cand", [128, 256], F32)
    tv = sb("tv", [128, 16], F32)
    tp_ = sb("tp", [128, 16], U32)
    hi_u = sb("hi_u", [128, 16], U32)
    lo_u = sb("lo_u", [128, 16], U32)
    hif = sb("hif", [128, 16], F32)
    lof = sb("lof", [128, 16], F32)
    eq = sb("eq", [128, 16, 16], F32)
    sel1 = sb("sel1", [128, 16], F32)
    sel2 = sb("sel2", [128, 16], F32)
    eidf = sb("eidf", [128, 128], F32)
    eidx = sb("eidx", [128, 128], I32)
    gat = sb("gat", [128, 128], F32)
    nm = sb("nm", [128, 1], F32)
    gs = sb("gs", [128, 1], F32)
    actv = sb("actv", [128, 128], F32)
    coef = sb("coef", [128, 128], F32)
    NBUF = 3
    ub = [sb(f"ub{i}", [128, 1024], F32) for i in range(NBUF)]
    vb = [sb(f"vb{i}", [128, 1024], F32) for i in range(NBUF)]
    t1 = ub[0]
    t2 = vb[0]
    pacc = sb("pacc", [128, 1024], F32)
    iota16 = CF("iota16")

    def tile(t):
        dma("sp", lambda e: e.dma_start(out=xt[:, :], in_=io["x_own"][t * 128:(t + 1) * 128, :]), writes=[xt.b])
        dma("sp", lambda e: e.dma_start(
            out=ysT[:, :, :],
            in_=io["ysb_own"].rearrange("(sc s2) d t -> (s2 d) sc t", s2=2)[:, :, t * 128:(t + 1) * 128]),
            reads=[io["own_b"]], writes=[ysT.b])
        dma("sp", lambda e: e.dma_start(
            out=og[:, :, :],
            in_=io["ogdn_own"][:, t * 128:(t + 1) * 128, :].rearrange("s p e -> p s e")),
            reads=[io["own_b"]], writes=[og.b])

        def norm_to_T(src, gsc_ap, gb_, sh_ap, shb_, dstT):
            op("act", lambda e: e.activation(out=xn[:, :], in_=src[:, :], func=AF.Square, accum_out=ssq[:, 0:1]),
               reads=[src.b], writes=[xn.b, ssq.b])
            op("act", lambda e: e.activation(out=rstd[:, :], in_=ssq[:, :], func=AF.Sqrt, scale=1.0 / D, bias=CF("eps")),
               reads=[ssq.b, cf.b], writes=[rstd.b])
            op("dve", lambda e: e.reciprocal(out=rstd[:, :], in_=rstd[:, :]), reads=[rstd.b], writes=[rstd.b])
            op("dve", lambda e: e.tensor_scalar(out=xn[:, :], in0=src[:, :], scalar1=rstd[:, 0:1], scalar2=None, op0=ALU.mult),
               reads=[src.b, rstd.b], writes=[xn.b])
            tp = bank()
            tpv = tp[:, :].bitcast(BF16)
            for c in range(8):
                op("pe", lambda e, c=c: e.transpose(out=tpv[:, c * 128:(c + 1) * 128], in_=xn[:, c * 128:(c + 1) * 128], identity=CB("ident")),
                   reads=[xn.b, cb.b], writes=[tp.b])
            for c in range(8):
                op("dve", lambda e, c=c: e.tensor_scalar(out=dstT[:, c, :], in0=tpv[:, c * 128:(c + 1) * 128], scalar1=gsc_ap(c),
                                                         scalar2=sh_ap(c), op0=ALU.mult, op1=ALU.add),
                   reads=[tp.b, gb_, shb_], writes=[dstT.b])

        norm_to_T(xt, lambda c: G1[:, c, 2:3], G1.b, lambda c: modT[:, c, 2:3], modT.b, hT)
        pz = bank()
        for c in range(8):
            op("pe", lambda e, c=c: e.matmul(pz[:, :], lhsT=hT[:, c, :], rhs=Wzg[:, c, 0:512], start=(c == 0), stop=(c == 7)),
               reads=[hT.b, Wzg.b], writes=[pz.b])
        op("act", lambda e: e.activation(out=sz[:, :], in_=pz[:, :], func=AF.Silu), reads=[pz.b], writes=[sz.b])
        for g in range(4):
            pgt = bank()
            for c in range(8):
                op("pe", lambda e, c=c, g=g, pgt=pgt: e.matmul(pgt[:, :], lhsT=hT[:, c, :], rhs=Wzg[:, c, 512 + g * 512:1024 + g * 512],
                                                              start=(c == 0), stop=(c == 7)), reads=[hT.b, Wzg.b], writes=[pgt.b])
            op("act", lambda e, g=g, pgt=pgt: e.activation(out=gts[:, g * 512:(g + 1) * 512], in_=pgt[:, :], func=AF.Sigmoid),
               reads=[pgt.b], writes=[gts.b])
        ot = og[:, :, :]
        op("dve", lambda e: e.tensor_tensor(out=osq[:, :, :], in0=ot, in1=ot, op=ALU.mult), reads=[og.b], writes=[osq.b])
        op("dve", lambda e: e.tensor_reduce(out=oss[:, :], in_=osq[:, :, :], axis=AX.X, op=ALU.add), reads=[osq.b], writes=[oss.b])
        op("act", lambda e: e.activation(out=oss[:, :], in_=oss[:, :], func=AF.Sqrt, scale=1.0 / 64, bias=CF("eps")),
           reads=[oss.b, cf.b], writes=[oss.b])
        op("dve", lambda e: e.reciprocal(out=oss[:, :], in_=oss[:, :]), reads=[oss.b], writes=[oss.b])
        op("dve", lambda e: e.tensor_tensor(out=on[:, :, :], in0=ot, in1=bc3(oss[:, :], 64), op=ALU.mult), reads=[og.b, oss.b], writes=[on.b])
        op("dve", lambda e: e.tensor_tensor(out=on[:, :, :], in0=on[:, :, :], in1=bcmid(onw[:, :], 8), op=ALU.mult),
           reads=[on.b, onw.b], writes=[on.b])
        op("dve", lambda e: e.tensor_tensor(out=onb[:, :], in0=on[:, :, :].rearrange("p h e -> p (h e)"), in1=sz[:, :], op=ALU.mult),
           reads=[on.b, sz.b], writes=[onb.b])
        pyg = bank()
        pygv = pyg[:, :].bitcast(BF16)
        for c in range(4):
            op("pe", lambda e, c=c: e.transpose(out=pygv[:, c * 128:(c + 1) * 128], in_=onb[:, c * 128:(c + 1) * 128], identity=CB("ident")),
               reads=[onb.b, cb.b], writes=[pyg.b])
        op("act", lambda e: e.copy(out=ygT[:, :, :].rearrange("p c t -> p (c t)"), in_=pygv[:, 0:512]), reads=[pyg.b], writes=[ygT.b])
        for hf in range(2):
            cs = slice(hf * 512, (hf + 1) * 512)
            p1 = bank()
            for c in range(4):
                op("pe", lambda e, c=c, p1=p1, cs=cs: e.matmul(p1[:, :], lhsT=ysT[:, c, :], rhs=Wsb[:, c, cs],
                                                              start=(c == 0), stop=(c == 3)), reads=[ysT.b, Wsb.b], writes=[p1.b])
            op("dve", lambda e, p1=p1, cs=cs, hf=hf: e.tensor_tensor(out=t1[:, cs], in0=p1[:, :], in1=gts[:, hf * 512:(hf + 1) * 512], op=ALU.mult),
               reads=[p1.b, gts.b], writes=[t1.b])
            p2 = bank()
            for c in range(4):
                op("pe", lambda e, c=c, p2=p2, cs=cs: e.matmul(p2[:, :], lhsT=ygT[:, c, :], rhs=Wgd[:, c, cs],
                                                              start=(c == 0), stop=(c == 3)), reads=[ygT.b, Wgd.b], writes=[p2.b])
            op("dve", lambda e, p2=p2, cs=cs, hf=hf: e.tensor_tensor(out=t2[:, cs], in0=p2[:, :], in1=gts[:, 1024 + hf * 512:1024 + (hf + 1) * 512],
                                                                    op=ALU.mult), reads=[p2.b, gts.b], writes=[t2.b])
        op("dve", lambda e: e.tensor_tensor(out=mg[:, :], in0=t1[:, :], in1=t2[:, :], op=ALU.add), reads=[t1.b, t2.b], writes=[mg.b])
        pm_ = bank()
        pmv = pm_[:, :].bitcast(BF16)
        for c in range(8):
            op("pe", lambda e, c=c: e.transpose(out=pmv[:, c * 128:(c + 1) * 128], in_=mg[:, c * 128:(c + 1) * 128], identity=CB("ident")),
               reads=[mg.b, cb.b], writes=[pm_.b])
        op("act", lambda e: e.copy(out=mT[:, :, :].rearrange("p c t -> p (c t)"), in_=pmv[:, :]), reads=[pm_.b], writes=[mT.b])
        for hf in range(2):
            cs = slice(hf * 512, (hf + 1) * 512)
            po = bank()
            for c in range(8):
                op("pe", lambda e, c=c, po=po, cs=cs: e.matmul(po[:, :], lhsT=mT[:, c, :], rhs=Wo[:, c, cs], start=(c == 0), stop=(c == 7)),
                   reads=[mT.b, Wo.b], writes=[po.b])
            op("dve", lambda e, po=po, cs=cs: e.tensor_tensor(out=t1[:, cs], in0=po[:, :], in1=gate_b[0][:, cs], op=ALU.mult),
               reads=[po.b, gate_b[0].b], writes=[t1.b])
        op("dve", lambda e: e.tensor_tensor(out=x1[:, :], in0=t1[:, :], in1=xt[:, :], op=ALU.add), reads=[t1.b, xt.b], writes=[x1.b])
        norm_to_T(x1, lambda c: G2[:, c:c + 1], G2.b, lambda c: modT[:, 24 + c, 2:3], modT.b, h2T)
        ph = bank()
        phv = ph[:, :].bitcast(BF16)
        for c in range(8):
            op("pe", lambda e, c=c: e.transpose(out=phv[:, c * 128:(c + 1) * 128], in_=h2T[:, c, :], identity=CB("ident")),
               reads=[h2T.b, cb.b], writes=[ph.b])
        op("act", lambda e: e.copy(out=h2[:, :], in_=phv[:, :]), reads=[ph.b], writes=[h2.b])
        for q4 in range(4):
            pq = bank()
            for cc in range(4):
                c16 = q4 * 4 + cc
                for c in range(8):
                    op("pe", lambda e, c=c, cc=cc, c16=c16, pq=pq: e.matmul(pq[:, cc * 128:(cc + 1) * 128], lhsT=Wq[:, c, c16 * 128:(c16 + 1) * 128],
                                                                           rhs=h2T[:, c, :], start=(c == 0), stop=(c == 7)),
                       reads=[Wq.b, h2T.b], writes=[pq.b])
            op("act", lambda e, q4=q4, pq=pq: e.copy(out=qTt[:, q4 * 4:(q4 + 1) * 4, :].rearrange("p c t -> p (c t)"), in_=pq[:, :]),
               reads=[pq.b], writes=[qTt.b])
        for q4 in range(4):
            pscr = bank()
            for cc in range(4):
                c16 = q4 * 4 + cc
                op("pe", lambda e, cc=cc, c16=c16, pscr=pscr: e.matmul(pscr[:, cc * 128:(cc + 1) * 128], lhsT=qTt[:, c16, :], rhs=skT[:, c16, :],
                                                                      start=True, stop=True), reads=[qTt.b, skT.b], writes=[pscr.b])
            op("act", lambda e, q4=q4, pscr=pscr: e.copy(out=sc[:, q4 * 4:(q4 + 1) * 4, :].rearrange("p c t -> p (c t)"), in_=pscr[:, :]),
               reads=[pscr.b], writes=[sc.b])

        def top16(src_ap, srcb, n, vals_ap, valsb, idx_ap, idxb):
            op("dve", lambda e: e.max(out=vals_ap[:, 0:8], in_=src_ap), reads=[srcb], writes=[valsb])
            op("dve", lambda e: e.max_index(out=idx_ap[:, 0:8], in_max=vals_ap[:, 0:8], in_values=src_ap), reads=[srcb, valsb], writes=[idxb])
            op("dve", lambda e: e.match_replace(out=sc2[:, 0:n], in_to_replace=vals_ap[:, 0:8], in_values=src_ap, imm_value=-1e30),
               reads=[srcb, valsb], writes=[sc2.b])
            op("dve", lambda e: e.max(out=vals_ap[:, 8:16], in_=sc2[:, 0:n]), reads=[sc2.b], writes=[valsb])
            op("dve", lambda e: e.max_index(out=idx_ap[:, 8:16], in_max=vals_ap[:, 8:16], in_values=sc2[:, 0:n]), reads=[sc2.b, valsb], writes=[idxb])

        for c16 in range(16):
            top16(sc[:, c16, :], sc.b, 128, v16[:, c16, :], v16.b, i16[:, c16, :], i16.b)
        op("dve", lambda e: e.tensor_copy(out=i16f[:, :, :], in_=i16[:, :, :]), reads=[i16.b], writes=[i16f.b])
        for h in range(8):
            a, b_ = 2 * h, 2 * h + 1
            op("dve", lambda e, a=a, b_=b_: e.tensor_tensor(out=cand[:, :].rearrange("p (i j) -> p i j", j=16), in0=bc3(v16[:, a, :], 16),
                                                           in1=bcmid(v16[:, b_, :], 16), op=ALU.add), reads=[v16.b], writes=[cand.b])
            top16(cand[:, :], cand.b, 256, tv[:, :], tv.b, tp_[:, :], tp_.b)
            op("dve", lambda e: e.tensor_single_scalar(out=hi_u[:, :], in_=tp_[:, :], scalar=4, op=ALU.logical_shift_right),
               reads=[tp_.b], writes=[hi_u.b])
            op("dve", lambda e: e.tensor_single_scalar(out=lo_u[:, :], in_=tp_[:, :], scalar=15, op=ALU.bitwise_and),
               reads=[tp_.b], writes=[lo_u.b])
            op("dve", lambda e: e.tensor_copy(out=hif[:, :], in_=hi_u[:, :]), reads=[hi_u.b], writes=[hif.b])
            op("dve", lambda e: e.tensor_copy(out=lof[:, :], in_=lo_u[:, :]), reads=[lo_u.b], writes=[lof.b])
            for (pf, src_i, dsel) in ((hif, a, sel1), (lof, b_, sel2)):
                op("dve", lambda e, pf=pf: e.tensor_tensor(out=eq[:, :, :], in0=bcmid(iota16, 16), in1=bc3(pf[:, :], 16), op=ALU.is_equal),
                   reads=[cf.b, pf.b], writes=[eq.b])
                op("dve", lambda e, src_i=src_i: e.tensor_tensor(out=eq[:, :, :], in0=eq[:, :, :], in1=bcmid(i16f[:, src_i, :], 16), op=ALU.mult),
                   reads=[eq.b, i16f.b], writes=[eq.b])
                op("dve", lambda e, dsel=dsel: e.tensor_reduce(out=dsel[:, :], in_=eq[:, :, :], axis=AX.X, op=ALU.add),
                   reads=[eq.b], writes=[dsel.b])
            op("dve", lambda e, h=h: e.scalar_tensor_tensor(out=eidf[:, h * 16:(h + 1) * 16], in0=sel1[:, :], scalar=128.0, in1=sel2[:, :],
                                                           op0=ALU.mult, op1=ALU.add), reads=[sel1.b, sel2.b], writes=[eidf.b])
            op("dve", lambda e: e.tensor_scalar(out=nm[:, :], in0=tv[:, 0:1], scalar1=-1.0, scalar2=None, op0=ALU.mult),
               reads=[tv.b], writes=[nm.b])
            op("act", lambda e, h=h: e.activation(out=gat[:, h * 16:(h + 1) * 16], in_=tv[:, :], func=AF.Exp, bias=nm[:, 0:1], accum_out=gs[:, 0:1]),
               reads=[tv.b, nm.b], writes=[gat.b, gs.b])
            op("dve", lambda e: e.reciprocal(out=gs[:, :], in_=gs[:, :]), reads=[gs.b], writes=[gs.b])
            op("dve", lambda e, h=h: e.tensor_scalar(out=gat[:, h * 16:(h + 1) * 16], in0=gat[:, h * 16:(h + 1) * 16], scalar1=gs[:, 0:1],
                                                    scalar2=None, op0=ALU.mult), reads=[gat.b, gs.b], writes=[gat.b])
        op("dve", lambda e: e.tensor_copy(out=eidx[:, :], in_=eidf[:, :]), reads=[eidf.b], writes=[eidx.b])
        for k in range(128):
            U = ub[k % NBUF]
            dma("pool", lambda e, k=k, U=U: e.indirect_dma_start(out=U[:, :], out_offset=None, in_=io["peer_u"][:, :],
                                                                in_offset=bass.IndirectOffsetOnAxis(ap=eidx[:, k:k + 1], axis=0)),
                reads=[eidx.b], writes=[U.b])
            op("dve", lambda e, k=k, U=U: e.scalar_tensor_tensor(out=pacc[:, :], in0=U[:, :], scalar=1.0, in1=h2[:, :],
                                                                op0=ALU.mult, op1=ALU.mult, accum_out=actv[:, k:k + 1]),
               reads=[U.b, h2.b], writes=[pacc.b, actv.b])
        op("act", lambda e: e.activation(out=coef[:, :], in_=actv[:, :], func=AF.Gelu), reads=[actv.b], writes=[coef.b])
        op("dve", lambda e: e.tensor_tensor(out=coef[:, :], in0=coef[:, :], in1=gat[:, :], op=ALU.mult), reads=[coef.b, gat.b], writes=[coef.b])
        for k in range(128):
            V = vb[k % NBUF]
            dma("pool", lambda e, k=k, V=V: e.indirect_dma_start(out=V[:, :], out_offset=None, in_=io["peer_v"][:, :],
                                                                in_offset=bass.IndirectOffsetOnAxis(ap=eidx[:, k:k + 1], axis=0)),
                reads=[eidx.b], writes=[V.b])
            if k == 0:
                op("dve", lambda e, V=V: e.tensor_scalar(out=pacc[:, :], in0=V[:, :], scalar1=coef[:, 0:1], scalar2=None, op0=ALU.mult),
                   reads=[V.b, coef.b], writes=[pacc.b])
            else:
                op("dve", lambda e, k=k, V=V: e.scalar_tensor_tensor(out=pacc[:, :], in0=V[:, :], scalar=coef[:, k:k + 1], in1=pacc[:, :],
                                                                    op0=ALU.mult, op1=ALU.add), reads=[V.b, coef.b, pacc.b], writes=[pacc.b])
        op("dve", lambda e: e.tensor_tensor(out=pacc[:, :], in0=pacc[:, :], in1=gate_b[1][:, :], op=ALU.mult),
           reads=[pacc.b, gate_b[1].b], writes=[pacc.b])
        op("dve", lambda e: e.tensor_tensor(out=pacc[:, :], in0=pacc[:, :], in1=x1[:, :], op=ALU.add), reads=[pacc.b, x1.b], writes=[pacc.b])
        dma("sp", lambda e: e.dma_start(out=io["out"][t * 128:(t + 1) * 128, :], in_=pacc[:, :]), reads=[pacc.b], writes=[io["out_b"]])

    for t in range(NT):
        tile(t)


def build_full(S):
    TT = 2 * S
    TOK = TT // 8
    nc = bass.Bass("TRN2", target_bir_lowering=False)
    io = {}

    def din(name, shape, dt=F32):
        io[name] = nc.dram_tensor(name, list(shape), dt, kind="ExternalInput").ap()

    din("consts", CONST_ARR.shape)
    din("x_all", [TT, D])
    din("cT", [128, 8, 3]); din("badaT", [128, 48]); din("n1wT", [128, 8]); din("w_ada", [1024, 6144])
    din("w_head", [1024, 386]); din("hvec", [128, 8]); din("hvec2", [128, 4])
    din("x_own", [TOK, D]); din("w_zg", [1024, 2560]); din("w_psb", [512, 1024]); din("w_pgdn", [512, 1024])
    din("w_o", [1024, 1024]); din("peer_wq", [1024, 2048]); din("skT", [128, 16, 128]); din("b_ada_row", [1, 6144])
    din("n2wT", [128, 8]); din("onw", [128, 64]); din("peer_u", [16384, 1024]); din("peer_v", [16384, 1024])
    io["out"] = nc.dram_tensor("out", [TOK, D], F32, kind="ExternalOutput").ap()
    ysb_x = nc.dram_tensor("ysb_x", [8, 64, TOK], BF16, kind="Internal").ap()
    ogdn_x = nc.dram_tensor("ogdn_x", [8, TOK, 64], BF16, kind="Internal").ap()
    io["ysb_g"] = nc.dram_tensor("ysb_g", [64, 64, TOK], BF16, kind="Internal").ap()
    io["ogdn_g"] = nc.dram_tensor("ogdn_g", [64, TOK, 64], BF16, kind="Internal").ap()
    io["ysb_own"] = nc.dram_tensor("ysb_own", [8, 64, TOK], BF16, kind="Internal").ap()
    io["ogdn_own"] = nc.dram_tensor("ogdn_own", [8, TOK, 64], BF16, kind="Internal").ap()
    io["own_b"] = Buf("own")
    io["ysb_out_b"] = Buf("ysb_x"); io["ogdn_out_b"] = Buf("ogdn_x"); io["xchg_b"] = Buf("xchg"); io["out_b"] = Buf("out")
    piece = min(TOK, NQ)

    def ysb_dst(m, ysb):
        r = []
        for p in range(NQ // piece):
            t0 = m * NQ + p * piece
            j, off = t0 // TOK, t0 % TOK
            r.append((ysb_x[j, :, off:off + piece], ysb[:, p * piece:(p + 1) * piece]))
        return r

    def ogdn_dst(m, otm):
        r = []
        nch = piece // 64
        for p in range(NQ // piece):
            t0 = m * NQ + p * piece
            j, off = t0 // TOK, t0 % TOK
            r.append((ogdn_x[j, off:off + piece, :].rearrange("(c t) e -> t c e", t=64), otm[:, p * nch:(p + 1) * nch, :]))
        return r

    io["ysb_dst"] = ysb_dst
    io["ogdn_dst"] = ogdn_dst
    with contextlib.ExitStack() as outer:
        sch = Sched(nc, outer)
        stackA = contextlib.ExitStack()
        A = build_A(nc, S, sch, stackA, io, outer=outer)
        sch.op("pool", lambda e: e.collective_compute("AllGather", ALU.bypass, replica_groups=[list(range(8))],
                                                      ins=[ysb_x.rearrange("j d t -> (j d) t")],
                                                      outs=[io["ysb_g"].rearrange("r d t -> (r d) t")]),
               reads=[io["ysb_out_b"]], writes=[io["xchg_b"]])
        sch.op("pool", lambda e: e.collective_compute("AllGather", ALU.bypass, replica_groups=[list(range(8))],
                                                      ins=[ogdn_x.rearrange("j t e -> (j t) e")],
                                                      outs=[io["ogdn_g"].rearrange("r t e -> (r t) e")]),
               reads=[io["ogdn_out_b"]], writes=[io["xchg_b"]])
        sch.full_barrier()
        stackA.close()
        stackB = contextlib.ExitStack()
        build_B(nc, S, sch, stackB, io, A)
        sch.wait_all("sp", [io["out_b"]])
        sch.full_barrier()
        sch.emit()
        stackB.close()
    return nc


def make_inputs(inp, S):
    TT = 2 * S
    TOK = TT // 8
    f32 = np.float32
    x = np.ascontiguousarray(np.asarray(inp["x"], f32).reshape(TT, D))
    c = np.asarray(inp["c"], f32)
    w_in = np.asarray(inp["w_in"], f32)[0]
    cw = np.asarray(inp["gdn_conv_w"], f32)[0]

    def fm(v, nch):
        return np.ascontiguousarray(np.asarray(v, f32).reshape(nch, 128).T)

    shared = {
        "consts": CONST_ARR, "x_all": x, "badaT": fm(inp["b_ada"][0], 48), "n1wT": fm(inp["norm1_w"][0], 8),
        "w_ada": np.ascontiguousarray(np.asarray(inp["w_ada"], f32)[0]),
        "w_zg": np.ascontiguousarray(w_in[:, 3088:5648]),
        "w_psb": np.ascontiguousarray(np.asarray(inp["w_proj_sb"], f32)[0]),
        "w_pgdn": np.ascontiguousarray(np.asarray(inp["w_proj_gdn"], f32)[0]),
        "w_o": np.ascontiguousarray(np.asarray(inp["w_o"], f32)[0]),
        "peer_wq": np.ascontiguousarray(np.asarray(inp["peer_w_q"], f32)[0]),
        "skT": np.ascontiguousarray(np.asarray(inp["peer_sub_keys"], f32)[0].transpose(3, 0, 1, 2).reshape(128, 16, 128)),
        "b_ada_row": np.ascontiguousarray(np.asarray(inp["b_ada"], f32)[0][None, :]),
        "n2wT": fm(inp["norm2_w"][0], 8),
        "onw": np.ascontiguousarray(np.tile(np.asarray(inp["gdn_o_norm_w"], f32)[0][None, :], (128, 1))),
        "peer_u": np.ascontiguousarray(np.asarray(inp["peer_u"], f32)[0]),
        "peer_v": np.ascontiguousarray(np.asarray(inp["peer_v"], f32)[0]),
    }
    maps = []
    for core in range(8):
        h = core
        ob = (core * TOK) // S
        d = dict(shared)
        c3 = np.stack([c[0], c[1], c[ob]], axis=-1)
        d["cT"] = np.ascontiguousarray(c3.reshape(8, 128, 3).transpose(1, 0, 2))
        hs = slice(h * 64, (h + 1) * 64)
        cols = [w_in[:, 0:512][:, hs], w_in[:, 512:1024][:, hs], w_in[:, 1024:1536][:, hs],
                w_in[:, 1536:2048][:, hs], w_in[:, 2048:2560][:, hs], w_in[:, 2560:3072][:, hs],
                w_in[:, 3072 + h:3073 + h], w_in[:, 3080 + h:3081 + h]]
        d["w_head"] = np.ascontiguousarray(np.concatenate(cols, axis=1))
        hv = np.zeros((128, 8), f32)
        hv[:, 0] = np.tile(np.asarray(inp["sb_q_norm_w"], f32)[0], 2)
        hv[:, 1] = np.tile(np.asarray(inp["sb_k_norm_w"], f32)[0], 2)
        hv[:, 2] = np.asarray(inp["gdn_A_log"], f32)[0, h]
        hv[:, 3] = np.asarray(inp["gdn_dt_bias"], f32)[0, h]
        hv[:64, 4:8] = cw[:, h * 64:(h + 1) * 64].T
        hv[64:, 4:8] = cw[:, 512 + h * 64:512 + (h + 1) * 64].T
        d["hvec"] = hv
        hv2 = np.zeros((128, 4), f32)
        hv2[:64] = cw[:, 1024 + h * 64:1024 + (h + 1) * 64].T
        d["hvec2"] = hv2
        d["x_own"] = np.ascontiguousarray(x[core * TOK:(core + 1) * TOK])
        maps.append(d)
    return maps


_NC_CACHE = {}


def kernel(**inputs):
    S = int(np.asarray(inputs["x"]).shape[1])
    if S not in _NC_CACHE:
        _NC_CACHE[S] = build_full(S)
    nc = _NC_CACHE[S]
    maps = make_inputs(inputs, S)
    res = run_bass_kernel_spmd(nc, maps, core_ids=list(range(8)))
    out = np.concatenate([np.asarray(res.results[i]["out"], np.float32) for i in range(8)], axis=0)
    return out.reshape(2, S, D)
```
